# Optimizing a Trainium2 kernel written in Bass

```python
import math
import jax, jax.numpy as jnp
from jax import lax
import numpy as np

D_MODEL = 2048
BATCH = 4
SEQ = 4096
DEPTH = 2

N_ATT_HEADS = 8
ATT_HALF_DIM = 64
ATT_V_DIM = 2 * ATT_HALF_DIM
D_ATT = N_ATT_HEADS * ATT_V_DIM
Q_BLOCK = 128
D_CONV = D_MODEL // 2
CONV_WIDTH = 31
D_IN = 3 * D_ATT + 2 * D_CONV + 2 * D_MODEL
N_EXPERTS = 32
TOP_K = 4
D_FF = D_MODEL
SWIGLU_LIMIT = 7.0
SWIGLU_ALPHA = 1.702
MOE_BLOCK = 128
LN_EPS = 1e-5
DEEPNORM_ALPHA = (2 * DEPTH) ** 0.25
DEEPNORM_BETA = (8 * DEPTH) ** -0.25

kernel_name = 'hybrid_diffattn_conformer_moe_deepnorm'


def layer_norm(x, g, b):
    xf = x.astype(jnp.float32)
    mu = jnp.mean(xf, axis=-1, keepdims=True)
    var = jnp.mean(jnp.square(xf - mu), axis=-1, keepdims=True)
    return ((xf - mu) * lax.rsqrt(var + LN_EPS)).astype(x.dtype) * g + b


def rms_norm(x, g):
    xf = x.astype(jnp.float32)
    return (xf * lax.rsqrt(jnp.mean(xf * xf, axis=-1, keepdims=True) + LN_EPS)).astype(x.dtype) * g


def alibi_slopes(n_heads):
    return jnp.asarray(2.0 ** (-8.0 * np.arange(1, n_heads + 1) / n_heads), dtype=jnp.float32)


def diff_attention(q, k, v, lam, slopes):
    B, S = q.shape[0], q.shape[1]
    n_blocks = S // Q_BLOCK
    scale = ATT_HALF_DIM ** -0.5
    q_blocks = q.reshape(B, n_blocks, Q_BLOCK, N_ATT_HEADS, 2, ATT_HALF_DIM).transpose(1, 0, 2, 3, 4, 5)
    pos_k = jnp.arange(S)

    def one_block(args):
        q_blk, start = args
        s = jnp.einsum('bqhmd,bkhmd->bhmqk', q_blk, k,
                       preferred_element_type=jnp.float32) * scale
        pos_q = start + jnp.arange(Q_BLOCK)
        dist = jnp.abs(pos_q[:, None] - pos_k[None, :]).astype(jnp.float32)
        s = s - slopes[None, :, None, None, None] * dist
        p = jax.nn.softmax(s, axis=-1)
        a = p[:, :, 0] - lam * p[:, :, 1]
        return jnp.einsum('bhqk,bkhe->bqhe', a.astype(v.dtype), v)

    starts = jnp.arange(n_blocks) * Q_BLOCK
    o = lax.map(one_block, (q_blocks, starts))
    return o.transpose(1, 0, 2, 3, 4).reshape(B, S, N_ATT_HEADS, ATT_V_DIM)


def conformer_conv(u, w_dw, b_dw, g_ln, b_ln):
    val, gate = jnp.split(u, 2, axis=-1)
    h = val * jax.nn.sigmoid(gate)
    pad = CONV_WIDTH // 2
    h = lax.conv_general_dilated(h, w_dw, window_strides=(1,), padding=[(pad, pad)],
                                 dimension_numbers=('NWC', 'WIO', 'NWC'),
                                 feature_group_count=D_CONV) + b_dw
    h = layer_norm(h, g_ln, b_ln)
    return jax.nn.silu(h)


def mixer(h, lam_init, w_in, b_gate, lq1, lk1, lq2, lk2, subln_g, w_dw, b_dw,
          cln_g, cln_b, w_pa, w_pc, b_pc, w_out, b_out, slopes):
    B, S, _ = h.shape
    proj = jnp.einsum('bsd,de->bse', h, w_in)
    splits = [D_ATT, 2 * D_ATT, 3 * D_ATT, 3 * D_ATT + 2 * D_CONV]
    q, k, v, u, g = jnp.split(proj, splits, axis=-1)
    q = q.reshape(B, S, N_ATT_HEADS, 2, ATT_HALF_DIM)
    k = k.reshape(B, S, N_ATT_HEADS, 2, ATT_HALF_DIM)
    v = v.reshape(B, S, N_ATT_HEADS, ATT_V_DIM)
    lam = (jnp.exp(jnp.sum(lq1.astype(jnp.float32) * lk1.astype(jnp.float32)))
           - jnp.exp(jnp.sum(lq2.astype(jnp.float32) * lk2.astype(jnp.float32))) + lam_init)
    o = diff_attention(q, k, v, lam, slopes)
    o = rms_norm(o, subln_g) * (1.0 - lam_init)
    y_att = jnp.einsum('bsa,ad->bsd', o.reshape(B, S, D_ATT), w_pa)
    c = conformer_conv(u, w_dw, b_dw, cln_g, cln_b)
    y_conv = jnp.einsum('bsc,cd->bsd', c, w_pc) + b_pc
    gates = jax.nn.sigmoid(g + b_gate)
    g_att, g_conv = jnp.split(gates, 2, axis=-1)
    merged = g_att * y_att + g_conv * y_conv
    return jnp.einsum('bsd,de->bse', merged, w_out) + b_out


def moe(x2, w_router, b_router, w_gu, b_gu, w_dn, b_dn):
    N = x2.shape[0]
    logits = (x2 @ w_router + b_router).astype(jnp.float32)
    top_val, top_idx = lax.top_k(logits, TOP_K)
    gate = jax.nn.softmax(top_val, axis=-1)
    e_flat = top_idx.reshape(-1)
    tok_flat = jnp.repeat(jnp.arange(N, dtype=jnp.int32), TOP_K)
    w_flat = gate.reshape(-1)
    order = jnp.argsort(e_flat)
    e_s, tok_s, w_s = e_flat[order], tok_flat[order], w_flat[order]
    counts = jnp.bincount(e_flat, length=N_EXPERTS)
    padded = ((counts + MOE_BLOCK - 1) // MOE_BLOCK) * MOE_BLOCK
    start = jnp.cumsum(counts) - counts
    pend = jnp.cumsum(padded)
    pstart = pend - padded
    rows = pstart[e_s] + (jnp.arange(N * TOP_K) - start[e_s])
    n_rows = N * TOP_K + N_EXPERTS * MOE_BLOCK
    n_blocks = n_rows // MOE_BLOCK
    row_tok = jnp.zeros((n_rows,), jnp.int32).at[rows].set(tok_s)
    row_w = jnp.zeros((n_rows,), jnp.float32).at[rows].set(w_s)
    block_e = jnp.minimum(jnp.searchsorted(pend, jnp.arange(n_blocks) * MOE_BLOCK, side='right'),
                          N_EXPERTS - 1)

    def one_block(args):
        tok, wt, e = args
        xb = x2[tok]
        gu = xb @ w_gu[e] + b_gu[e]
        g, u = jnp.split(gu, 2, axis=-1)
        g = jnp.minimum(g, SWIGLU_LIMIT)
        u = jnp.clip(u, -SWIGLU_LIMIT, SWIGLU_LIMIT)
        hid = g * jax.nn.sigmoid(SWIGLU_ALPHA * g) * (u + 1.0)
        y = hid @ w_dn[e] + b_dn[e]
        return y * wt[:, None].astype(y.dtype)

    y_rows = lax.map(one_block, (row_tok.reshape(n_blocks, MOE_BLOCK),
                                 row_w.reshape(n_blocks, MOE_BLOCK), block_e))
    return jnp.zeros_like(x2).at[row_tok].add(y_rows.reshape(n_rows, x2.shape[1]))


def setup_inputs(seed: int = 0) -> dict:
    key = jax.random.key(seed)
    keys = list(jax.random.split(key, 40))
    L, D, E, F = DEPTH, D_MODEL, N_EXPERTS, D_FF

    def nrm(i, shape, scale):
        return jax.random.normal(keys[i], shape, jnp.float32) * scale

    x = nrm(0, (BATCH, SEQ, D), 1.0)
    ln_in_g = 1.0 + nrm(1, (D,), 0.02)
    ln_in_b = nrm(2, (D,), 0.02)
    w_qk = nrm(3, (L, D, 2 * D_ATT), D ** -0.5)
    w_v = nrm(4, (L, D, D_ATT), DEEPNORM_BETA * D ** -0.5)
    w_rest = nrm(5, (L, D, 2 * D_CONV + 2 * D), D ** -0.5)
    w_in = jnp.concatenate([w_qk, w_v, w_rest], axis=-1)
    b_gate = nrm(6, (L, 2 * D), 0.02)
    lambda_q1 = nrm(7, (L, ATT_HALF_DIM), 0.1)
    lambda_k1 = nrm(8, (L, ATT_HALF_DIM), 0.1)
    lambda_q2 = nrm(9, (L, ATT_HALF_DIM), 0.1)
    lambda_k2 = nrm(10, (L, ATT_HALF_DIM), 0.1)
    subln_g = 1.0 + nrm(11, (L, ATT_V_DIM), 0.02)
    w_dw = nrm(12, (L, CONV_WIDTH, 1, D_CONV), CONV_WIDTH ** -0.5)
    b_dw = nrm(13, (L, D_CONV), 0.02)
    conv_ln_g = 1.0 + nrm(14, (L, D_CONV), 0.02)
    conv_ln_b = nrm(15, (L, D_CONV), 0.02)
    w_pa = nrm(16, (L, D_ATT, D), DEEPNORM_BETA * D_ATT ** -0.5)
    w_pc = nrm(17, (L, D_CONV, D), DEEPNORM_BETA * D_CONV ** -0.5)
    b_pc = nrm(18, (L, D), 0.02)
    w_out = nrm(19, (L, D, D), DEEPNORM_BETA * D ** -0.5)
    b_out = nrm(20, (L, D), 0.02)
    ln1_g = 1.0 + nrm(21, (L, D), 0.02)
    ln1_b = nrm(22, (L, D), 0.02)
    w_router = nrm(23, (L, D, E), D ** -0.5)
    b_router = nrm(24, (L, E), 0.01)
    w_gu = nrm(25, (L, E, D, 2 * F), D ** -0.5)
    b_gu = nrm(26, (L, E, 2 * F), 0.02)
    w_dn = nrm(27, (L, E, F, D), DEEPNORM_BETA * F ** -0.5)
    b_dn = nrm(28, (L, E, D), 0.02)
    ln2_g = 1.0 + nrm(29, (L, D), 0.02)
    ln2_b = nrm(30, (L, D), 0.02)
    return {'x': x, 'ln_in_g': ln_in_g, 'ln_in_b': ln_in_b, 'w_in': w_in, 'b_gate': b_gate,
            'lambda_q1': lambda_q1, 'lambda_k1': lambda_k1, 'lambda_q2': lambda_q2,
            'lambda_k2': lambda_k2, 'subln_g': subln_g, 'w_dw': w_dw, 'b_dw': b_dw,
            'conv_ln_g': conv_ln_g, 'conv_ln_b': conv_ln_b, 'w_pa': w_pa, 'w_pc': w_pc,
            'b_pc': b_pc, 'w_out': w_out, 'b_out': b_out, 'ln1_g': ln1_g, 'ln1_b': ln1_b,
            'w_router': w_router, 'b_router': b_router, 'w_gu': w_gu, 'b_gu': b_gu,
            'w_dn': w_dn, 'b_dn': b_dn, 'ln2_g': ln2_g, 'ln2_b': ln2_b}


def reference(x, ln_in_g, ln_in_b, w_in, b_gate, lambda_q1, lambda_k1, lambda_q2, lambda_k2,
              subln_g, w_dw, b_dw, conv_ln_g, conv_ln_b, w_pa, w_pc, b_pc, w_out, b_out,
              ln1_g, ln1_b, w_router, b_router, w_gu, b_gu, w_dn, b_dn, ln2_g, ln2_b):
    B, S, D = x.shape
    slopes = alibi_slopes(N_ATT_HEADS)
    h = layer_norm(x, ln_in_g, ln_in_b)
    for l in range(DEPTH):
        lam_init = 0.8 - 0.6 * math.exp(-0.3 * l)
        mix = mixer(h, lam_init, w_in[l], b_gate[l], lambda_q1[l], lambda_k1[l],
                    lambda_q2[l], lambda_k2[l], subln_g[l], w_dw[l], b_dw[l],
                    conv_ln_g[l], conv_ln_b[l], w_pa[l], w_pc[l], b_pc[l],
                    w_out[l], b_out[l], slopes)
        h = layer_norm(DEEPNORM_ALPHA * h + mix, ln1_g[l], ln1_b[l])
        ff = moe(h.reshape(B * S, D), w_router[l], b_router[l], w_gu[l], b_gu[l],
                 w_dn[l], b_dn[l]).reshape(B, S, D)
        h = layer_norm(DEEPNORM_ALPHA * h + ff, ln2_g[l], ln2_b[l])
    return h
```

```python
import contextlib
import math
import numpy as np
import concourse.bass as bass
import concourse.mybir as mybir
from concourse.bass_utils import run_bass_kernel_spmd

F32 = mybir.dt.float32
BF16 = mybir.dt.bfloat16
I32 = mybir.dt.int32
AF = mybir.ActivationFunctionType
ALU = mybir.AluOpType
AX = mybir.AxisListType

D = 2048
S = 4096
T = 2048
NH = 8
DIN = 9216
NE = 32
FF = 2048
LN_EPS = 1e-5
ALPHA = 4.0 ** 0.25
NB = 512

ENGS = ("pe", "act", "dve", "pool", "sp")


class Sched:
    def __init__(self, nc, sems):
        self.nc = nc
        self.free_sems = list(sems)
        self.sem_of = {}
        self.sig_count = {}
        self.dma_pos = {}
        self.reset_phase()

    def reset_phase(self):
        self.ops = []
        self.last_writer = {}
        self.readers = {}
        self.chan_of_token = {}

    def _sem(self, dom):
        if dom not in self.sem_of:
            self.sem_of[dom] = self.free_sems.pop()
        return self.sem_of[dom]

    def add(self, eng, fn, reads=(), writes=(), dma=False):
        i = len(self.ops)
        deps = set()
        for r in reads:
            w = self.last_writer.get(r)
            if w is not None:
                deps.add(w)
        for w in writes:
            lw = self.last_writer.get(w)
            if lw is not None:
                deps.add(lw)
            deps.update(self.readers.get(w, ()))
        deps.discard(i)
        for r in reads:
            self.readers.setdefault(r, []).append(i)
        for w in writes:
            self.last_writer[w] = i
            self.readers[w] = []
        if dma:
            tok = writes[0]
            if tok not in self.chan_of_token:
                self.chan_of_token[tok] = len(self.chan_of_token)
            dom = ("dma", self.chan_of_token[tok])
        else:
            dom = eng
        self.ops.append(dict(eng=eng, fn=fn, deps=deps, dma=dma, dom=dom))
        return i

    def dma(self, out, in_, reads, writes, eng="sp"):
        return self.add(eng, lambda e: e.dma_start(out=out, in_=in_), reads, writes, dma=True)

    def emit(self, name=None):
        ops = self.ops
        if not ops:
            return
        dma_ids = [i for i, o in enumerate(ops) if o["dma"]]
        ops.append(dict(eng="sp", fn=None, deps=set(dma_ids), dma=False, dom="sp"))
        cpos = {}
        for o in ops:
            d = o["dom"]
            if o["dma"]:
                self.dma_pos[d] = self.dma_pos.get(d, 0) + 1
                o["pos"] = self.dma_pos[d]
            else:
                cpos[d] = cpos.get(d, 0) + 1
                o["pos"] = cpos[d]
        known = {e: {} for e in ENGS}
        clocks = {}
        needed = set()
        for o in ops:
            needed.update(o["deps"])
        sig = {}
        for i, o in enumerate(ops):
            E = o["eng"]
            k = known[E]
            waits = {}
            for d in sorted(o["deps"]):
                Dop = ops[d]
                dom = Dop["dom"]
                if dom == "pe" and E == "pe":
                    continue
                if k.get(dom, 0) >= Dop["pos"]:
                    continue
                waits[dom] = max(waits.get(dom, 0), Dop["pos"])
                k[dom] = Dop["pos"]
                for dm, p in clocks[d]:
                    if k.get(dm, 0) < p:
                        k[dm] = p
            o["waits"] = waits
            for dom, p in waits.items():
                if not isinstance(dom, tuple):
                    sig[(dom, p)] = True
            if i in needed:
                c = [(dm, k.get(dm, 0)) for dm in ENGS if k.get(dm, 0) > 0 and dm != o["dom"]]
                if not o["dma"]:
                    c.append((o["dom"], o["pos"]))
                clocks[i] = c
        target = {}
        for e in ENGS:
            base = self.sig_count.get(e, 0)
            n = 0
            for o in ops:
                if o["dom"] == e:
                    if (e, o["pos"]) in sig:
                        n += 1
                        o["signal"] = True
                        target[(e, o["pos"])] = base + n
            self.sig_count[e] = base + n
        per_eng = {e: [] for e in ENGS}
        for o in ops:
            per_eng[o["eng"]].append(o)
        nc = self.nc

        def run(eng_obj, lst):
            for o in lst:
                for dom, p in o["waits"].items():
                    if isinstance(dom, tuple):
                        eng_obj.wait_ge(self._sem(dom), 16 * p)
                    else:
                        eng_obj.wait_ge(self._sem(dom), target[(dom, p)])
                if o["fn"] is None:
                    continue
                ins = o["fn"](eng_obj)
                if o["dma"]:
                    ins.then_inc(self._sem(o["dom"]), 16)
                elif o.get("signal"):
                    ins.then_inc(self._sem(o["dom"]), 1)

        with nc.Block() as block:
            @block.tensor
            def _(e):
                run(e, per_eng["pe"])

            @block.scalar
            def _(e):
                run(e, per_eng["act"])

            @block.vector
            def _(e):
                run(e, per_eng["dve"])

            @block.gpsimd
            def _(e):
                run(e, per_eng["pool"])

            @block.sync
            def _(e):
                run(e, per_eng["sp"])
        self.reset_phase()


class WLoader:
    def __init__(self, S_, es, nc, name, KC, NCOL, nbuf=2):
        self.S = S_
        self.KC, self.NCOL, self.nbuf = KC, NCOL, nbuf
        self.name = name
        self.wbf = [es.enter_context(nc.sbuf_tensor(f"{name}_bf{i}", [128, KC, NCOL], BF16)) for i in range(nbuf)]
        self.n = 0

    def load(self, W, col_ranges, kc=None):
        kc = kc or self.KC
        i = self.n % self.nbuf
        self.n += 1
        wbf = self.wbf[i]
        Wv = W.rearrange("(c p) n -> p c n", p=128)
        o = 0
        toks = []
        for j, (c0, ncols) in enumerate(col_ranges):
            tk = (self.name, i, j)
            self.S.dma(wbf[:, 0:kc, o:o + ncols], Wv[:, 0:kc, c0:c0 + ncols], reads=[], writes=[tk], eng="pool")
            toks.append(tk)
            o += ncols
        return wbf, toks


def build_layer(layer, debug=False, upto=9):
    lam_init = 0.8 - 0.6 * math.exp(-0.3 * layer)
    first = layer == 0
    nc = bass.Bass("TRN2", target_bir_lowering=False)
    dk = "ExternalOutput" if debug else "Internal"

    def din(name, shape, dt=F32):
        return nc.dram_tensor(name, list(shape), dt, kind="ExternalInput").ap()

    def dscr(name, shape, dt):
        return nc.dram_tensor(name, list(shape), dt, kind=dk).ap()

    xT = din("xT", [D, S])
    ln_in_g = din("ln_in_g", [128, 16]); ln_in_b = din("ln_in_b", [128, 16])
    w_in = din("w_in", [D, DIN])
    b_gate = din("b_gate", [128, 32])
    lam_vecs = din("lam_vecs", [4, 64])
    subln_g = din("subln_g", [1, 128])
    w_dwT = din("w_dwT", [128, 8, 31]); b_dw = din("b_dw", [128, 8])
    cln_g = din("cln_g", [128, 8]); cln_b = din("cln_b", [128, 8])
    w_pa = din("w_pa", [1024, D]); w_pc = din("w_pc", [1024, D]); b_pc = din("b_pc", [128, 16])
    w_out = din("w_out", [D, D]); b_out = din("b_out", [128, 16])
    ln1_g = din("ln1_g", [128, 16]); ln1_b = din("ln1_b", [128, 16])
    w_router = din("w_router", [D, NE]); b_router = din("b_router", [1, NE])
    if upto >= 5:
        w_gu = din("w_gu", [NE, D, 2 * FF]); b_gu = din("b_gu", [128, NE, 32])
        w_dn = din("w_dn", [NE, FF, D]); b_dn = din("b_dn", [NE, D])
    ln2_g = din("ln2_g", [128, 16]); ln2_b = din("ln2_b", [128, 16])
    outT = nc.dram_tensor("outT", [D, T], F32, kind="ExternalOutput").ap()

    hres = dscr("hres", [D, T], F32)
    QTd = dscr("QTd", [NH, 128, T], BF16)
    KTd = dscr("KTd", [NH, 128, S], BF16)
    Vd = dscr("Vd", [NH, 128, 32, 128], BF16)
    Gd = dscr("Gd", [8, 128, 2080], F32)
    ONTd = dscr("ONTd", [1024, T], BF16)
    CTd = dscr("CTd", [1024, T], BF16)
    X1d = dscr("X1d", [D, T], F32)
    X1Bd = dscr("X1Bd", [D, T], BF16)
    GTd = dscr("GTd", [NE, T], F32)

    with contextlib.ExitStack() as top:
        sems = [top.enter_context(nc.semaphore(f"s{i}")) for i in range(90)]
        Sc = Sched(nc, sems)
        identf = top.enter_context(nc.sbuf_tensor("identf", [128, 128], F32))
        identb = top.enter_context(nc.sbuf_tensor("identb", [128, 128], BF16))
        onesm = top.enter_context(nc.sbuf_tensor("onesm", [128, 128], F32))
        onesc = top.enter_context(nc.sbuf_tensor("onesc", [128, 128], F32))
        lng = top.enter_context(nc.sbuf_tensor("lng", [128, 8, 16], F32))
        bgate = top.enter_context(nc.sbuf_tensor("bgate", [128, 32], F32))
        cvec = top.enter_context(nc.sbuf_tensor("cvec", [128, 3, 8], F32))
        lamt = top.enter_context(nc.sbuf_tensor("lamt", [128, 8], F32))
        with contextlib.ExitStack() as es:
            ii = es.enter_context(nc.sbuf_tensor("ii", [128, 128], I32))
            lv = es.enter_context(nc.sbuf_tensor("lv", [128, 4, 64], F32))
            lp = es.enter_context(nc.sbuf_tensor("lp", [128, 2, 64], F32))
            Sc.add("pool", lambda e: e.iota(ii[:], pattern=[[1, 128]], base=0, channel_multiplier=-1), [], ["ii"])
            Sc.add("dve", lambda e: e.tensor_scalar(out=identf[:], in0=ii[:], scalar1=0.0, scalar2=None, op0=ALU.is_equal), ["ii"], ["identf"])
            Sc.add("dve", lambda e: e.tensor_copy(out=identb[:], in_=identf[:]), ["identf"], ["identb"])
            Sc.add("pool", lambda e: e.memset(onesm[:], 1.0 / 2048.0), [], ["onesm"])
            Sc.add("pool", lambda e: e.memset(onesc[:], 1.0 / 1024.0), [], ["onesc"])
            for j, src in enumerate([ln_in_g, ln_in_b, ln1_g, ln1_b, ln2_g, ln2_b, b_pc, b_out]):
                Sc.dma(lng[:, j, :], src[:, :], [], [("lng", j)])
            Sc.dma(bgate[:], b_gate[:, :], [], ["bgate"])
            for j, src in enumerate([b_dw, cln_g, cln_b]):
                Sc.dma(cvec[:, j, :], src[:, :], [], [("cvec", j)])
            Sc.dma(lv[:].rearrange("p a b -> p (a b)"), lam_vecs.rearrange("a b -> (a b)").partition_broadcast(128), [], ["lv"])
            Sc.add("dve", lambda e: e.tensor_tensor(out=lp[:, 0, :], in0=lv[:, 0, :], in1=lv[:, 1, :], op=ALU.mult), ["lv"], ["lp0"])
            Sc.add("dve", lambda e: e.tensor_tensor(out=lp[:, 1, :], in0=lv[:, 2, :], in1=lv[:, 3, :], op=ALU.mult), ["lv"], ["lp1"])
            Sc.add("dve", lambda e: e.reduce_sum(out=lamt[:, 0:1], in_=lp[:, 0, :], axis=AX.X), ["lp0"], ["l0"])
            Sc.add("dve", lambda e: e.reduce_sum(out=lamt[:, 1:2], in_=lp[:, 1, :], axis=AX.X), ["lp1"], ["l1"])
            Sc.add("act", lambda e: e.activation(out=lamt[:, 2:4], in_=lamt[:, 0:2], func=AF.Exp), ["l0", "l1"], ["l23"])
            Sc.add("dve", lambda e: e.tensor_tensor(out=lamt[:, 4:5], in0=lamt[:, 3:4], in1=lamt[:, 2:3], op=ALU.subtract), ["l23"], ["l4"])
            Sc.add("dve", lambda e: e.tensor_scalar(out=lamt[:, 5:6], in0=lamt[:, 4:5], scalar1=-lam_init, scalar2=None, op0=ALU.add), ["l4"], ["nlam"])
            Sc.emit()
        nlam = lamt[:, 5:6]

        def g16(j, c):
            return lng[:, j, c:c + 1]

        def ln_block(xt, nch, ones_t, gcol, bcol, ps_mean, ps_ex2, sq, st, tag, xtok):
            for c in range(nch):
                Sc.add("pe", lambda e, c=c: e.matmul(ps_mean[:], lhsT=ones_t[:], rhs=xt[:, c, :], start=(c == 0), stop=(c == nch - 1)),
                       [xtok(c)], [(tag, "pm")])
            for c in range(nch):
                b = c % 2
                Sc.add("act", lambda e, c=c, b=b: e.activation(out=sq[:, b, :], in_=xt[:, c, :], func=AF.Square), [xtok(c)], [(tag, "sq", b)])
                Sc.add("pe", lambda e, c=c, b=b: e.matmul(ps_ex2[:], lhsT=ones_t[:], rhs=sq[:, b, :], start=(c == 0), stop=(c == nch - 1)),
                       [(tag, "sq", b)], [(tag, "pe2")])
            Sc.add("act", lambda e: e.activation(out=st[:, 0, :], in_=ps_mean[:], func=AF.Copy), [(tag, "pm")], [(tag, "mean")])
            Sc.add("dve", lambda e: e.tensor_tensor(out=st[:, 1, :], in0=st[:, 0, :], in1=st[:, 0, :], op=ALU.mult), [(tag, "mean")], [(tag, "msq")])
            Sc.add("dve", lambda e: e.tensor_tensor(out=st[:, 1, :], in0=ps_ex2[:], in1=st[:, 1, :], op=ALU.subtract), [(tag, "pe2"), (tag, "msq")], [(tag, "msq")])
            Sc.add("dve", lambda e: e.tensor_scalar(out=st[:, 1, :], in0=st[:, 1, :], scalar1=0.0, scalar2=LN_EPS, op0=ALU.max, op1=ALU.add), [(tag, "msq")], [(tag, "msq")])
            Sc.add("act", lambda e: e.activation(out=st[:, 2, :], in_=st[:, 1, :], func=AF.Sqrt), [(tag, "msq")], [(tag, "rstd")])
            Sc.add("dve", lambda e: e.reciprocal(out=st[:, 2, :], in_=st[:, 2, :]), [(tag, "rstd")], [(tag, "rstd")])
            for c in range(nch):
                eng = "dve" if c % 2 == 0 else "pool"
                Sc.add(eng, lambda e, c=c: e.tensor_tensor(out=xt[:, c, :], in0=xt[:, c, :], in1=st[:, 0, :], op=ALU.subtract), [xtok(c), (tag, "mean")], [xtok(c)])
                Sc.add(eng, lambda e, c=c: e.tensor_tensor(out=xt[:, c, :], in0=xt[:, c, :], in1=st[:, 2, :], op=ALU.mult), [xtok(c), (tag, "rstd")], [xtok(c)])
                if gcol is not None:
                    Sc.add(eng, lambda e, c=c: e.tensor_scalar(out=xt[:, c, :], in0=xt[:, c, :], scalar1=gcol(c), scalar2=bcol(c), op0=ALU.mult, op1=ALU.add), [xtok(c)], [xtok(c)])

        xTv = xT.rearrange("(c p) t -> p c t", p=128)
        with contextlib.ExitStack() as es:
            sb = lambda n, s, d: es.enter_context(nc.sbuf_tensor(n, s, d))
            pst = lambda n, s, d: es.enter_context(nc.psum_tensor(n, s, d))
            xin = [sb(f"xin{i}", [128, 16, NB], F32) for i in range(2)]
            hb = [sb(f"hb{i}", [128, 16, NB], BF16) for i in range(2)]
            sq = sb("sq", [128, 2, NB], F32); st = sb("st", [128, 3, NB], F32)
            WL = WLoader(Sc, es, nc, "w1", 16, 256, nbuf=3)
            ps_mean = pst("ps_mean", [128, NB], F32); ps_ex2 = pst("ps_ex2", [128, NB], F32)
            pp = [pst(f"pp{i}", [128, NB], F32) for i in range(4)]
            ptr = pst("ptr", [128, 512], BF16)
            qst = [sb(f"qst{i}", [128, NB], BF16) for i in range(4)]
            vT = [sb(f"vT{i}", [128, NB], BF16) for i in range(2)]
            vtok = sb("vtok", [128, 4, 8, 128], BF16)
            gv = [sb(f"gv{i}", [128, NB], F32) for i in range(2)]
            gs = [sb(f"gs{i}", [128, NB], F32) for i in range(2)]
            zpad = sb("zpad", [128, 8, 16], F32)
            Sc.add("pool", lambda e: e.memset(zpad[:], 0.0), [], ["zpad"])
            Sc.dma(Gd.rearrange("c p t -> p c t")[:, :, 0:15], zpad[:, :, 0:15], ["zpad"], ["gpad"])
            cnt = dict(pp=0, qst=0, vT=0, g=0)
            for tb in range(8):
                b = tb % 2
                t0 = tb * NB
                own = tb < 4
                X = xin[b]
                xtok = lambda c, b=b: ("xin", b, c)
                Sc.dma(X[:, :, :], xTv[:, :, t0:t0 + NB], [], [xtok(c) for c in range(16)])
                if first:
                    ln_block(X, 16, onesm, lambda c: g16(0, c), lambda c: g16(1, c), ps_mean, ps_ex2, sq, st, "ln0", xtok)
                    if own:
                        Sc.dma(hres.rearrange("(c p) t -> p c t", p=128)[:, :, t0:t0 + NB], X[:, :, :], [xtok(c) for c in range(16)], [("hres", tb)])
                for c in range(16):
                    Sc.add("act", lambda e, c=c, b=b, X=X: e.activation(out=hb[b][:, c, :], in_=X[:, c, :], func=AF.Copy), [xtok(c)], [("hb", b, c)])
                tiles = []
                if own:
                    tiles += [("q", j, [(j * 256, 256)]) for j in range(4)]
                tiles += [("k", j, [(1024 + j * 256, 256)]) for j in range(4)]
                tiles += [("v", j, [(2048 + j * 256, 256)]) for j in range(4)]
                if own or tb == 4:
                    tiles += [("u", c, [(3072 + c * 128, 128), (4096 + c * 128, 128)]) for c in range(8)]
                for kind, j, ranges in tiles:
                    N = NB if (own or kind != "u") else 16
                    wbf, wtok = WL.load(w_in, ranges)
                    for half in range(2):
                        pi = cnt["pp"] % 4; cnt["pp"] += 1
                        ps = pp[pi]
                        for kc in range(16):
                            Sc.add("pe", lambda e, ps=ps, wbf=wbf, kc=kc, half=half, N=N, b=b: e.matmul(
                                ps[:, 0:N], lhsT=wbf[:, kc, half * 128:(half + 1) * 128], rhs=hb[b][:, kc, 0:N],
                                start=(kc == 0), stop=(kc == 15)), wtok + [("hb", b, kc)], [("pp", pi)])
                        if kind in ("q", "k"):
                            h = 2 * j + half
                            qi = cnt["qst"] % 4; cnt["qst"] += 1
                            sc = 0.125 if kind == "q" else 1.0
                            Sc.add("act", lambda e, ps=ps, qi=qi, sc=sc: e.activation(out=qst[qi][:], in_=ps[:], func=AF.Copy, scale=sc), [("pp", pi)], [("qst", qi)])
                            dst = QTd if kind == "q" else KTd
                            Sc.dma(dst[h, :, t0:t0 + NB], qst[qi][:], [("qst", qi)], [("qkstore", qi)])
                        elif kind == "v":
                            h = 2 * j + half
                            vi = cnt["vT"] % 2; cnt["vT"] += 1
                            Sc.add("dve", lambda e, ps=ps, vi=vi: e.tensor_copy(out=vT[vi][:], in_=ps[:]), [("pp", pi)], [("vT", vi)])
                            for i in range(4):
                                Sc.add("pe", lambda e, i=i, vi=vi: e.transpose(out=ptr[:, i * 128:(i + 1) * 128], in_=vT[vi][:, i * 128:(i + 1) * 128], identity=identb[:]),
                                       [("vT", vi), "identb"], [("ptr", i)])
                            Sc.add("dve", lambda e, h=h: e.tensor_copy(out=vtok[:, :, h, :], in_=ptr[:].rearrange("p (i e) -> p i e", i=4)),
                                   [("ptr", i) for i in range(4)], [("vtok", h)])
                        else:
                            gi = cnt["g"] % 2
                            if half == 0:
                                Sc.add("act", lambda e, ps=ps, gi=gi, N=N: e.activation(out=gv[gi][:, 0:N], in_=ps[:, 0:N], func=AF.Copy), [("pp", pi)], [("gv", gi)])
                            else:
                                cnt["g"] += 1
                                Sc.add("act", lambda e, ps=ps, gi=gi, N=N: e.activation(out=gs[gi][:, 0:N], in_=ps[:, 0:N], func=AF.Sigmoid), [("pp", pi)], [("gs", gi)])
                                Sc.add("dve", lambda e, gi=gi, N=N: e.tensor_tensor(out=gv[gi][:, 0:N], in0=gv[gi][:, 0:N], in1=gs[gi][:, 0:N], op=ALU.mult), [("gv", gi), ("gs", gi)], [("gv", gi)])
                                Sc.dma(Gd[j, :, 15 + t0:15 + t0 + N], gv[gi][:, 0:N], [("gv", gi)], [("gstore", gi)])
                for i in range(4):
                    Sc.dma(Vd.rearrange("h p t e -> p t h e")[:, tb * 4 + i, :, :], vtok[:, i, :, :], [("vtok", h) for h in range(8)], [("vstore", i)])
            Sc.emit()
        if upto == 1:
            return nc

        with contextlib.ExitStack() as es:
            sb = lambda n, s, d: es.enter_context(nc.sbuf_tensor(n, s, d))
            pst = lambda n, s, d: es.enter_context(nc.psum_tensor(n, s, d))
            W0i = sb("W0i", [128, 6016], I32)
            W0 = sb("W0", [128, 6016], F32)
            Sc.add("pool", lambda e: e.iota(W0i[:], pattern=[[1, 6016]], base=-3968, channel_multiplier=-1), [], ["W0i"])
            Sc.add("act", lambda e: e.activation(out=W0[:], in_=W0i[:], func=AF.Abs), ["W0i"], ["W0"])
            gt = sb("gt", [128, 128], F32)
            Sc.dma(gt[:], subln_g[0:1, :].partition_broadcast(128).rearrange("p a e -> p (a e)"), [], ["gt"])
            KT = [sb(f"KT{i}", [128, S], BF16) for i in range(2)]
            QT = [sb(f"QT{i}", [128, T], BF16) for i in range(2)]
            Vt = [sb(f"Vt{i}", [128, 32, 132], BF16) for i in range(2)]
            ONT = [sb(f"ONT{i}", [128, T], BF16) for i in range(2)]
            for i in range(2):
                Sc.add("pool", lambda e, i=i: e.memset(Vt[i][:, :, 128:129], 1.0), [], [("Vones", i)])
            ssb = [sb(f"ssb{i}", [128, NB], F32) for i in range(2)]
            PT = [sb(f"PT{i}", [128, NB], BF16) for i in range(2)]
            o1 = sb("o1", [128, 4, 128], F32)
            od = [sb(f"od{i}", [128, 128], F32) for i in range(2)]
            osq = sb("osq", [128, 128], F32)
            onb = [sb(f"onb{i}", [128, 128], BF16) for i in range(2)]
            sm = [sb(f"sm{i}", [128, 8], F32) for i in range(2)]
            pS = [pst(f"pS{i}", [128, NB], F32) for i in range(2)]
            pO = [pst(f"pO{i}", [128, 512], F32) for i in range(4)]
            pT = pst("pT", [128, 256], BF16)
            it = 0
            nsm = 0
            ntr = 0
            for h in range(NH):
                hbuf = h % 2
                slope = 2.0 ** (-(h + 1))
                Sc.dma(KT[hbuf][:], KTd[h, :, :], [], [("KT", hbuf)])
                Sc.dma(QT[hbuf][:], QTd[h, :, :], [], [("QT", hbuf)])
                Sc.dma(Vt[hbuf][:, :, 0:128], Vd[h, :, :, :], [("Vones", hbuf)], [("Vt", hbuf)])
                for qb in range(4):
                    for m in range(2):
                        for kt in range(32):
                            si = it % 2; it += 1
                            Sc.add("pe", lambda e, si=si, m=m, kt=kt, qb=qb, hbuf=hbuf: e.matmul(
                                pS[si][:], lhsT=KT[hbuf][m * 64:(m + 1) * 64, kt * 128:(kt + 1) * 128],
                                rhs=QT[hbuf][m * 64:(m + 1) * 64, qb * NB:(qb + 1) * NB], start=True, stop=True),
                                [("KT", hbuf), ("QT", hbuf)], [("pS", si)])
                            off = qb * NB - kt * 128 + 3968
                            Sc.add("dve", lambda e, si=si, off=off, slope=slope: e.scalar_tensor_tensor(
                                out=ssb[si][:], in0=W0[:, off:off + NB], scalar=-slope, in1=pS[si][:], op0=ALU.mult, op1=ALU.add),
                                [("pS", si), "W0"], [("ssb", si)])
                            Sc.add("act", lambda e, si=si: e.activation(out=PT[si][:], in_=ssb[si][:], func=AF.Exp), [("ssb", si)], [("PT", si)])
                            for qi in range(4):
                                Sc.add("pe", lambda e, si=si, qi=qi, kt=kt, hbuf=hbuf: e.matmul(
                                    pO[qi][:, 0:129], lhsT=PT[si][:, qi * 128:(qi + 1) * 128], rhs=Vt[hbuf][:, kt, 0:129],
                                    start=(kt == 0), stop=(kt == 31)), [("PT", si), ("Vt", hbuf), ("Vones", hbuf)], [("pO", qi)])
                        for qi in range(4):
                            s_ = sm[nsm % 2]; stok = ("sm", nsm % 2); nsm += 1
                            Sc.add("dve", lambda e, qi=qi, s_=s_: e.reciprocal(out=s_[:, 0:1], in_=pO[qi][:, 128:129]), [("pO", qi)], [stok])
                            if m == 0:
                                Sc.add("dve", lambda e, qi=qi, s_=s_: e.tensor_scalar(out=o1[:, qi, :], in0=pO[qi][:, 0:128], scalar1=s_[:, 0:1], scalar2=None, op0=ALU.mult),
                                       [("pO", qi), stok], [("o1", qi)])
                            else:
                                oi = ntr % 2; ntr += 1
                                Sc.add("dve", lambda e, s_=s_: e.tensor_tensor(out=s_[:, 1:2], in0=s_[:, 0:1], in1=nlam, op=ALU.mult), [stok], [stok])
                                Sc.add("dve", lambda e, qi=qi, s_=s_, oi=oi: e.scalar_tensor_tensor(out=od[oi][:], in0=pO[qi][:, 0:128], scalar=s_[:, 1:2], in1=o1[:, qi, :], op0=ALU.mult, op1=ALU.add),
                                       [("pO", qi), stok, ("o1", qi)], [("od", oi)])
                                Sc.add("dve", lambda e, oi=oi: e.tensor_tensor(out=osq[:], in0=od[oi][:], in1=od[oi][:], op=ALU.mult), [("od", oi)], ["osq"])
                                Sc.add("dve", lambda e, s_=s_: e.reduce_sum(out=s_[:, 2:3], in_=osq[:], axis=AX.X), ["osq"], [stok])
                                Sc.add("dve", lambda e, s_=s_: e.tensor_scalar(out=s_[:, 3:4], in0=s_[:, 2:3], scalar1=1.0 / 128.0, scalar2=LN_EPS, op0=ALU.mult, op1=ALU.add), [stok], [stok])
                                Sc.add("act", lambda e, s_=s_: e.activation(out=s_[:, 4:5], in_=s_[:, 3:4], func=AF.Sqrt), [stok], [stok])
                                Sc.add("dve", lambda e, s_=s_: e.reciprocal(out=s_[:, 5:6], in_=s_[:, 4:5]), [stok], [stok])
                                Sc.add("dve", lambda e, s_=s_, oi=oi: e.tensor_scalar(out=od[oi][:], in0=od[oi][:], scalar1=s_[:, 5:6], scalar2=(1.0 - lam_init), op0=ALU.mult, op1=ALU.mult), [stok, ("od", oi)], [("od", oi)])
                                Sc.add("dve", lambda e, oi=oi: e.tensor_tensor(out=onb[oi][:], in0=od[oi][:], in1=gt[:], op=ALU.mult), [("od", oi), "gt"], [("onb", oi)])
                                Sc.add("pe", lambda e, oi=oi: e.transpose(out=pT[:, oi * 128:(oi + 1) * 128], in_=onb[oi][:], identity=identb[:]), [("onb", oi)], [("pT", oi)])
                                q0 = qb * NB + qi * 128
                                Sc.add("act", lambda e, oi=oi, q0=q0, hbuf=hbuf: e.activation(out=ONT[hbuf][:, q0:q0 + 128], in_=pT[:, oi * 128:(oi + 1) * 128], func=AF.Copy), [("pT", oi)], [("ONT", hbuf)])
                Sc.dma(ONTd[h * 128:(h + 1) * 128, :], ONT[hbuf][:], [("ONT", hbuf)], [("ONTst", hbuf)])
            Sc.emit()
        if upto == 2:
            return nc

        with contextlib.ExitStack() as es:
            sb = lambda n, s, d: es.enter_context(nc.sbuf_tensor(n, s, d))
            pst = lambda n, s, d: es.enter_context(nc.psum_tensor(n, s, d))
            Gt = [sb(f"Gt{i}", [128, 2080], F32) for i in range(2)]
            cv = sb("cv", [128, 8, T], F32)
            wt = sb("wt", [128, 8, 31], F32)
            sq = sb("sq3", [128, 2, NB], F32); st = sb("st3", [128, 3, NB], F32)
            cb = [sb(f"cb{i}", [128, NB], BF16) for i in range(2)]
            ps_mean = pst("ps_mean3", [128, NB], F32); ps_ex2 = pst("ps_ex23", [128, NB], F32)
            Sc.dma(wt[:], w_dwT[:, :, :], [], ["wt"])
            for c in range(8):
                g = Gt[c % 2]
                gtok = ("Gt", c % 2)
                Sc.dma(g[:, 0:2078], Gd[c, :, 0:2078], [], [gtok])
                for j in range(31):
                    for tb in range(4):
                        t0 = tb * NB
                        if j == 0:
                            Sc.add("dve", lambda e, c=c, g=g, t0=t0: e.tensor_scalar(out=cv[:, c, t0:t0 + NB], in0=g[:, t0:t0 + NB], scalar1=wt[:, c, 0:1], scalar2=cvec[:, 0, c:c + 1], op0=ALU.mult, op1=ALU.add),
                                   [gtok, "wt"], [("cv", c, tb)])
                        else:
                            Sc.add("dve", lambda e, c=c, g=g, t0=t0, j=j: e.scalar_tensor_tensor(out=cv[:, c, t0:t0 + NB], in0=g[:, t0 + j:t0 + j + NB], scalar=wt[:, c, j:j + 1], in1=cv[:, c, t0:t0 + NB], op0=ALU.mult, op1=ALU.add),
                                   [gtok, ("cv", c, tb)], [("cv", c, tb)])
            ncb = 0
            for tb in range(4):
                t0 = tb * NB
                xt = cv[:, :, t0:t0 + NB]
                xtok = lambda c, tb=tb: ("cv", c, tb)
                ln_block(xt, 8, onesc, None, None, ps_mean, ps_ex2, sq, st, "ln3", xtok)
                for c in range(8):
                    bi = ncb % 2; ncb += 1
                    Sc.add("act", lambda e, c=c, bi=bi, xt=xt: e.activation(out=cb[bi][:], in_=xt[:, c, :], func=AF.Silu, scale=cvec[:, 1, c:c + 1], bias=cvec[:, 2, c:c + 1]),
                           [xtok(c)], [("cb", bi)])
                    Sc.dma(CTd[c * 128:(c + 1) * 128, t0:t0 + NB], cb[bi][:], [("cb", bi)], [("cbst", bi)])
            Sc.emit()
        if upto == 3:
            return nc

        with contextlib.ExitStack() as es:
            sb = lambda n, s, d: es.enter_context(nc.sbuf_tensor(n, s, d))
            pst = lambda n, s, d: es.enter_context(nc.psum_tensor(n, s, d))
            hx = sb("hx", [128, 16, NB], F32)
            hbb = sb("hbb", [128, 16, NB], BF16)
            ont = sb("ont", [128, 8, NB], BF16); ct = sb("ct", [128, 8, NB], BF16)
            mg = sb("mg", [128, 16, NB], BF16)
            WL = WLoader(Sc, es, nc, "w4", 16, 128, nbuf=6)
            ga = [sb(f"ga{i}", [128, NB], F32) for i in range(2)]
            gc = [sb(f"gc{i}", [128, NB], F32) for i in range(2)]
            t1 = [sb(f"t1{i}", [128, NB], F32) for i in range(2)]
            t2 = [sb(f"t2{i}", [128, NB], F32) for i in range(2)]
            sq = sb("sq4", [128, 2, NB], F32); st = sb("st4", [128, 3, NB], F32)
            wr = sb("wr", [128, 16, NE], F32); brt = sb("brt", [128, NE], F32)
            lg = sb("lg", [128, NE], F32); mx8 = sb("mx8", [128, 8], F32); msk = sb("msk", [128, NE], F32)
            ex = sb("ex", [128, NE], F32); rs = sb("rs", [128, 4], F32); Gk = sb("Gk", [128, NE], F32)
            GTs = sb("GTs", [32, NB], F32)
            pg = [pst(f"pg{i}", [128, NB], F32) for i in range(2)]
            py = [pst(f"py{i}", [128, NB], F32) for i in range(2)]
            pm = pst("pm", [128, NB], F32)
            ps_mean = pst("ps_mean4", [128, NB], F32); ps_ex2 = pst("ps_ex24", [128, NB], F32)
            pr = pst("pr", [128, 512], F32)
            Sc.dma(wr[:], w_router.rearrange("(c p) n -> p c n", p=128), [], ["wr"])
            Sc.dma(brt[:], b_router[0:1, :].partition_broadcast(128).rearrange("p a e -> p (a e)"), [], ["brt"])
            hsrc = hres.rearrange("(c p) t -> p c t", p=128) if first else xTv
            for tb in range(4):
                t0 = tb * NB
                xtok = lambda c: ("hx", c)
                Sc.dma(hx[:, :, :], hsrc[:, :, t0:t0 + NB], [], [xtok(c) for c in range(16)])
                Sc.dma(ont[:, :, :], ONTd.rearrange("(c p) t -> p c t", p=128)[:, :, t0:t0 + NB], [], ["ont"])
                Sc.dma(ct[:, :, :], CTd.rearrange("(c p) t -> p c t", p=128)[:, :, t0:t0 + NB], [], ["ct"])
                for c in range(16):
                    Sc.add("act", lambda e, c=c: e.activation(out=hbb[:, c, :], in_=hx[:, c, :], func=AF.Copy), [xtok(c)], [("hbb", c)])
                for i in range(16):
                    p2 = i % 2
                    wa, ta = WL.load(w_in, [(5120 + i * 128, 128)])
                    for kc in range(16):
                        Sc.add("pe", lambda e, wa=wa, kc=kc: e.matmul(pg[0][:], lhsT=wa[:, kc, 0:128], rhs=hbb[:, kc, :], start=(kc == 0), stop=(kc == 15)), ta + [("hbb", kc)], ["pg0"])
                    Sc.add("act", lambda e, i=i, p2=p2: e.activation(out=ga[p2][:], in_=pg[0][:], func=AF.Sigmoid, bias=bgate[:, i:i + 1]), ["pg0", "bgate"], [("ga", p2)])
                    wc, tc = WL.load(w_in, [(5120 + 2048 + i * 128, 128)])
                    for kc in range(16):
                        Sc.add("pe", lambda e, wc=wc, kc=kc: e.matmul(pg[1][:], lhsT=wc[:, kc, 0:128], rhs=hbb[:, kc, :], start=(kc == 0), stop=(kc == 15)), tc + [("hbb", kc)], ["pg1"])
                    Sc.add("act", lambda e, i=i, p2=p2: e.activation(out=gc[p2][:], in_=pg[1][:], func=AF.Sigmoid, bias=bgate[:, 16 + i:17 + i]), ["pg1", "bgate"], [("gc", p2)])
                    wpa, tpa = WL.load(w_pa, [(i * 128, 128)], kc=8)
                    for kc in range(8):
                        Sc.add("pe", lambda e, wpa=wpa, kc=kc: e.matmul(py[0][:], lhsT=wpa[:, kc, 0:128], rhs=ont[:, kc, :], start=(kc == 0), stop=(kc == 7)), tpa + ["ont"], ["py0"])
                    wpc, tpc = WL.load(w_pc, [(i * 128, 128)], kc=8)
                    for kc in range(8):
                        Sc.add("pe", lambda e, wpc=wpc, kc=kc: e.matmul(py[1][:], lhsT=wpc[:, kc, 0:128], rhs=ct[:, kc, :], start=(kc == 0), stop=(kc == 7)), tpc + ["ct"], ["py1"])
                    Sc.add("dve", lambda e, p2=p2: e.tensor_tensor(out=t1[p2][:], in0=py[0][:], in1=ga[p2][:], op=ALU.mult), ["py0", ("ga", p2)], [("t1", p2)])
                    Sc.add("dve", lambda e, p2=p2, i=i: e.scalar_tensor_tensor(out=t2[p2][:], in0=py[1][:], scalar=g16(6, i), in1=gc[p2][:], op0=ALU.add, op1=ALU.mult), ["py1", ("gc", p2), ("lng", 6)], [("t2", p2)])
                    Sc.add("dve", lambda e, p2=p2, i=i: e.tensor_tensor(out=mg[:, i, :], in0=t1[p2][:], in1=t2[p2][:], op=ALU.add), [("t1", p2), ("t2", p2)], [("mg", i)])
                for i in range(16):
                    p2 = i % 2
                    wo, to = WL.load(w_out, [(i * 128, 128)])
                    for kc in range(16):
                        Sc.add("pe", lambda e, wo=wo, kc=kc: e.matmul(pm[:], lhsT=wo[:, kc, 0:128], rhs=mg[:, kc, :], start=(kc == 0), stop=(kc == 15)), to + [("mg", kc)], ["pm"])
                    Sc.add("act", lambda e, i=i, p2=p2: e.activation(out=t1[p2][:], in_=pm[:], func=AF.Identity, bias=g16(7, i)), ["pm", ("lng", 7)], [("t1", p2)])
                    Sc.add("dve", lambda e, i=i, p2=p2: e.scalar_tensor_tensor(out=hx[:, i, :], in0=hx[:, i, :], scalar=ALPHA, in1=t1[p2][:], op0=ALU.mult, op1=ALU.add), [xtok(i), ("t1", p2)], [xtok(i)])
                ln_block(hx, 16, onesm, lambda c: g16(2, c), lambda c: g16(3, c), ps_mean, ps_ex2, sq, st, "ln4", xtok)
                Sc.dma(X1d.rearrange("(c p) t -> p c t", p=128)[:, :, t0:t0 + NB], hx[:, :, :], [xtok(c) for c in range(16)], ["x1st"])
                for c in range(16):
                    Sc.add("act", lambda e, c=c: e.activation(out=hbb[:, c, :], in_=hx[:, c, :], func=AF.Copy), [xtok(c)], [("hbb", c)])
                Sc.dma(X1Bd.rearrange("(c p) t -> p c t", p=128)[:, :, t0:t0 + NB], hbb[:, :, :], [("hbb", c) for c in range(16)], ["x1bst"])
                for tt in range(4):
                    for kc in range(16):
                        Sc.add("pe", lambda e, tt=tt, kc=kc: e.matmul(pr[:, tt * 32:(tt + 1) * 32], lhsT=hx[:, kc, tt * 128:(tt + 1) * 128], rhs=wr[:, kc, :], start=(kc == 0), stop=(kc == 15)),
                               [xtok(kc), "wr"], [("pr", tt)])
                    Sc.add("dve", lambda e, tt=tt: e.tensor_tensor(out=lg[:], in0=pr[:, tt * 32:(tt + 1) * 32], in1=brt[:], op=ALU.add), [("pr", tt), "brt"], ["lg"])
                    Sc.add("dve", lambda e: e.max(out=mx8[:], in_=lg[:]), ["lg"], ["mx8"])
                    Sc.add("dve", lambda e: e.tensor_scalar(out=msk[:], in0=lg[:], scalar1=mx8[:, 3:4], scalar2=None, op0=ALU.is_ge), ["lg", "mx8"], ["msk"])
                    Sc.add("dve", lambda e: e.tensor_scalar(out=rs[:, 0:1], in0=mx8[:, 0:1], scalar1=-1.0, scalar2=None, op0=ALU.mult), ["mx8"], ["rs0"])
                    Sc.add("act", lambda e: e.activation(out=ex[:], in_=lg[:], func=AF.Exp, bias=rs[:, 0:1]), ["lg", "rs0"], ["ex"])
                    Sc.add("dve", lambda e: e.tensor_tensor(out=ex[:], in0=ex[:], in1=msk[:], op=ALU.mult), ["ex", "msk"], ["ex"])
                    Sc.add("dve", lambda e: e.reduce_sum(out=rs[:, 1:2], in_=ex[:], axis=AX.X), ["ex"], ["rs1"])
                    Sc.add("dve", lambda e: e.reciprocal(out=rs[:, 2:3], in_=rs[:, 1:2]), ["rs1"], ["rs2"])
                    Sc.add("dve", lambda e: e.tensor_scalar(out=Gk[:], in0=ex[:], scalar1=rs[:, 2:3], scalar2=None, op0=ALU.mult), ["ex", "rs2"], ["Gk"])
                    Sc.add("pe", lambda e: e.transpose(out=pr[0:32, 128:256], in_=Gk[:], identity=identf[:]), ["Gk", "identf"], ["prT"])
                    Sc.add("act", lambda e, tt=tt: e.activation(out=GTs[:, tt * 128:(tt + 1) * 128], in_=pr[0:32, 128:256], func=AF.Copy), ["prT"], ["GTs"])
                Sc.dma(GTd[:, t0:t0 + NB], GTs[:], ["GTs"], ["gtst"])
            Sc.emit()
        if upto == 4:
            return nc

        with contextlib.ExitStack() as es:
            sb = lambda n, s, d: es.enter_context(nc.sbuf_tensor(n, s, d))
            pst = lambda n, s, d: es.enter_context(nc.psum_tensor(n, s, d))
            x1b = sb("x1b", [128, 16, NB], BF16)
            yacc = sb("yacc", [128, 16, NB], F32)
            hid = sb("hid", [128, 16, NB], BF16)
            GT = sb("GT", [32, NB], F32)
            bdn = sb("bdn", [32, D], F32)
            bgu = sb("bgu", [128, NE, 32], F32)
            GB = [sb(f"GB{i}", [128, NB], F32) for i in range(2)]
            WG = WLoader(Sc, es, nc, "wg", 16, 512, nbuf=2)
            WD = WLoader(Sc, es, nc, "wd", 16, 256, nbuf=2)
            tg = [sb(f"tg{i}", [128, NB], F32) for i in range(2)]
            tsg = [sb(f"tsg{i}", [128, NB], F32) for i in range(2)]
            tu = [sb(f"tu{i}", [128, NB], F32) for i in range(2)]
            x1c = [sb(f"x1c{i}", [128, NB], F32) for i in range(2)]
            sq = sb("sq5", [128, 2, NB], F32); st = sb("st5", [128, 3, NB], F32)
            pgu = [pst(f"pgu{i}", [128, NB], F32) for i in range(4)]
            pdn = [pst(f"pdn{i}", [128, NB], F32) for i in range(2)]
            ps_mean = pst("ps_mean5", [128, NB], F32); ps_ex2 = pst("ps_ex25", [128, NB], F32)
            Sc.dma(bdn[:], b_dn[:, :], [], ["bdn"])
            Sc.dma(bgu[:], b_gu[:, :, :], [], ["bgu"])
            npg = 0; npd = 0; nt = 0
            for tb in range(4):
                t0 = tb * NB
                ytok = lambda c: ("yacc", c)
                Sc.dma(x1b[:, :, :], X1Bd.rearrange("(c p) t -> p c t", p=128)[:, :, t0:t0 + NB], [], [("x1b", c) for c in range(16)])
                Sc.dma(GT[:], GTd[:, t0:t0 + NB], [], ["GT"])
                for i in range(16):
                    pd = npd % 2; npd += 1
                    Sc.add("pe", lambda e, i=i, pd=pd: e.matmul(pdn[pd][:], lhsT=bdn[:, i * 128:(i + 1) * 128], rhs=GT[:], start=True, stop=True), ["bdn", "GT"], [("pdn", pd)])
                    Sc.add("act", lambda e, i=i, pd=pd: e.activation(out=yacc[:, i, :], in_=pdn[pd][:], func=AF.Copy), [("pdn", pd)], [ytok(i)])
                for ex_ in range(NE):
                    gb = GB[ex_ % 2]; gbt = ("GB", ex_ % 2)
                    Sc.dma(gb[:], GTd[ex_:ex_ + 1, t0:t0 + NB].partition_broadcast(128).rearrange("p a t -> p (a t)"), [], [gbt])
                    for j2 in range(8):
                        w, wt_ = WG.load(w_gu[ex_], [(j2 * 256, 256), (2048 + j2 * 256, 256)])
                        for jj in range(2):
                            j = j2 * 2 + jj
                            a = npg % 4; b_ = (npg + 1) % 4; npg += 2
                            for kc in range(16):
                                Sc.add("pe", lambda e, w=w, kc=kc, jj=jj, a=a: e.matmul(pgu[a][:], lhsT=w[:, kc, jj * 128:(jj + 1) * 128], rhs=x1b[:, kc, :], start=(kc == 0), stop=(kc == 15)),
                                       wt_ + [("x1b", kc)], [("pgu", a)])
                            for kc in range(16):
                                Sc.add("pe", lambda e, w=w, kc=kc, jj=jj, b_=b_: e.matmul(pgu[b_][:], lhsT=w[:, kc, 256 + jj * 128:256 + (jj + 1) * 128], rhs=x1b[:, kc, :], start=(kc == 0), stop=(kc == 15)),
                                       wt_ + [("x1b", kc)], [("pgu", b_)])
                            ti = nt % 2; nt += 1
                            Sc.add("dve", lambda e, a=a, ti=ti, ex_=ex_, j=j: e.tensor_scalar(out=tg[ti][:], in0=pgu[a][:], scalar1=bgu[:, ex_, j:j + 1], scalar2=7.0, op0=ALU.add, op1=ALU.min), [("pgu", a), "bgu"], [("tg", ti)])
                            Sc.add("act", lambda e, ti=ti: e.activation(out=tsg[ti][:], in_=tg[ti][:], func=AF.Sigmoid, scale=1.702), [("tg", ti)], [("tsg", ti)])
                            Sc.add("dve", lambda e, b_=b_, ti=ti, ex_=ex_, j=j: e.tensor_scalar(out=tu[ti][:], in0=pgu[b_][:], scalar1=bgu[:, ex_, 16 + j:17 + j], scalar2=7.0, op0=ALU.add, op1=ALU.min), [("pgu", b_), "bgu"], [("tu", ti)])
                            Sc.add("dve", lambda e, ti=ti: e.tensor_scalar(out=tu[ti][:], in0=tu[ti][:], scalar1=-7.0, scalar2=1.0, op0=ALU.max, op1=ALU.add), [("tu", ti)], [("tu", ti)])
                            Sc.add("dve", lambda e, ti=ti, gb=gb: e.tensor_tensor(out=tu[ti][:], in0=tu[ti][:], in1=gb[:], op=ALU.mult), [("tu", ti), gbt], [("tu", ti)])
                            Sc.add("dve", lambda e, ti=ti: e.tensor_tensor(out=tg[ti][:], in0=tg[ti][:], in1=tsg[ti][:], op=ALU.mult), [("tg", ti), ("tsg", ti)], [("tg", ti)])
                            Sc.add("dve", lambda e, ti=ti, j=j: e.tensor_tensor(out=hid[:, j, :], in0=tg[ti][:], in1=tu[ti][:], op=ALU.mult), [("tg", ti), ("tu", ti)], [("hid", j)])
                    for i2 in range(8):
                        w, wt_ = WD.load(w_dn[ex_], [(i2 * 256, 256)])
                        for ii_ in range(2):
                            i = i2 * 2 + ii_
                            pd = npd % 2; npd += 1
                            for jc in range(16):
                                Sc.add("pe", lambda e, w=w, jc=jc, ii_=ii_, pd=pd: e.matmul(pdn[pd][:], lhsT=w[:, jc, ii_ * 128:(ii_ + 1) * 128], rhs=hid[:, jc, :], start=(jc == 0), stop=(jc == 15)),
                                       wt_ + [("hid", jc)], [("pdn", pd)])
                            Sc.add("dve", lambda e, i=i, pd=pd: e.tensor_tensor(out=yacc[:, i, :], in0=pdn[pd][:], in1=yacc[:, i, :], op=ALU.add), [("pdn", pd), ytok(i)], [ytok(i)])
                for i in range(16):
                    xi = i % 2
                    Sc.dma(x1c[xi][:], X1d[i * 128:(i + 1) * 128, t0:t0 + NB], [], [("x1c", xi)])
                    Sc.add("dve", lambda e, i=i, xi=xi: e.scalar_tensor_tensor(out=yacc[:, i, :], in0=x1c[xi][:], scalar=ALPHA, in1=yacc[:, i, :], op0=ALU.mult, op1=ALU.add), [("x1c", xi), ytok(i)], [ytok(i)])
                ln_block(yacc, 16, onesm, lambda c: g16(4, c), lambda c: g16(5, c), ps_mean, ps_ex2, sq, st, "ln5", ytok)
                Sc.dma(outT.rearrange("(c p) t -> p c t", p=128)[:, :, t0:t0 + NB], yacc[:, :, :], [ytok(c) for c in range(16)], ["outst"])
            Sc.emit()
        return nc


def _colmajor(v, n):
    return np.ascontiguousarray(np.asarray(v, np.float32).reshape(n, 128).T)


def layer_params(inp, l, rev):
    w_dw = np.asarray(inp["w_dw"][l], np.float32).reshape(31, 1024)
    if rev:
        w_dw = w_dw[::-1]
    p = {
        "ln_in_g": _colmajor(inp["ln_in_g"], 16), "ln_in_b": _colmajor(inp["ln_in_b"], 16),
        "w_in": inp["w_in"][l], "b_gate": _colmajor(inp["b_gate"][l], 32),
        "lam_vecs": np.ascontiguousarray(np.stack([inp["lambda_q1"][l], inp["lambda_k1"][l], inp["lambda_q2"][l], inp["lambda_k2"][l]]).astype(np.float32)),
        "subln_g": np.ascontiguousarray(np.asarray(inp["subln_g"][l], np.float32).reshape(1, 128)),
        "w_dwT": np.ascontiguousarray(w_dw.T.reshape(8, 128, 31).transpose(1, 0, 2)),
        "b_dw": _colmajor(inp["b_dw"][l], 8), "cln_g": _colmajor(inp["conv_ln_g"][l], 8), "cln_b": _colmajor(inp["conv_ln_b"][l], 8),
        "w_pa": inp["w_pa"][l], "w_pc": inp["w_pc"][l], "b_pc": _colmajor(inp["b_pc"][l], 16),
        "w_out": inp["w_out"][l], "b_out": _colmajor(inp["b_out"][l], 16),
        "ln1_g": _colmajor(inp["ln1_g"][l], 16), "ln1_b": _colmajor(inp["ln1_b"][l], 16),
        "w_router": inp["w_router"][l], "b_router": np.ascontiguousarray(np.asarray(inp["b_router"][l], np.float32).reshape(1, NE)),
        "ln2_g": _colmajor(inp["ln2_g"][l], 16), "ln2_b": _colmajor(inp["ln2_b"][l], 16),
    }
    if "w_gu" in inp:
        p["w_gu"] = inp["w_gu"][l]
        p["b_gu"] = np.asarray(inp["b_gu"][l], np.float32).reshape(NE, 32, 128).transpose(2, 0, 1)
        p["w_dn"] = inp["w_dn"][l]
        p["b_dn"] = inp["b_dn"][l]
    return {k: np.ascontiguousarray(np.asarray(v, np.float32)) for k, v in p.items()}


def _declared_inputs(nc):
    return None


def core_xT(h_seq, core):
    if core % 2 == 1:
        h_seq = h_seq[::-1]
    return np.ascontiguousarray(h_seq.T)


_NC_CACHE = {}


def run_layer(inp, l, h):
    if l not in _NC_CACHE:
        _NC_CACHE[l] = build_layer(l)
    nc = _NC_CACHE[l]
    pf = layer_params(inp, l, False)
    pr = dict(pf)
    prr = layer_params(inp, l, True)
    pr["w_dwT"] = prr["w_dwT"]
    in_maps = []
    for core in range(8):
        m = dict(pf if core % 2 == 0 else pr)
        m["xT"] = core_xT(h[core // 2], core)
        in_maps.append(m)
    res = run_bass_kernel_spmd(nc, in_maps, core_ids=list(range(8)))
    out = np.empty_like(h)
    for core in range(8):
        o = res.results[core]["outT"].T
        bidx = core // 2
        if core % 2 == 0:
            out[bidx, 0:T] = o
        else:
            out[bidx, T:S] = o[::-1]
    return out


def kernel(**inputs):
    h = np.asarray(inputs["x"], np.float32)
    for l in range(2):
        h = run_layer(inputs, l, h)
    return h
```
